# Optimizing a Trainium2 kernel written in Bass

```python
import math
import jax
import jax.numpy as jnp
from jax import lax
import numpy as np

D_MODEL = 1024
BATCH = 8
SEQ = 2048
DEPTH = 4

GRID_W = 64
CTX_LEN = 256
HEAD_DIM = 64
ATTN_WIDTH = D_MODEL // 2
N_Q_HEADS = ATTN_WIDTH // HEAD_DIM
N_KV_HEADS = N_Q_HEADS // 4
KV_REP = N_Q_HEADS // N_KV_HEADS
KV_WIDTH = N_KV_HEADS * HEAD_DIM
FOURIER_WIDTH = D_MODEL - ATTN_WIDTH
N_FOURIER_GROUPS = 4
FOURIER_GROUP = FOURIER_WIDTH // N_FOURIER_GROUPS
MIX_WIDTH = ATTN_WIDTH + FOURIER_WIDTH
IN_WIDTH = ATTN_WIDTH + 2 * KV_WIDTH + FOURIER_WIDTH
ROT_PER_AXIS = HEAD_DIM // 2
ROPE_THETA = 10000.0
Q_BLOCK = 128
N_MOD = 6
D_FF_DENSE = ((8 * D_MODEL // 3 + 255) // 256) * 256
N_EXPERTS = 8
TOP_K = 2
D_FF_EXPERT = 7 * D_MODEL // 2
MOE_BLOCK = 256
EPS = 1e-6
N_DENSE_LAYERS = (DEPTH + 1) // 2
N_MOE_LAYERS = DEPTH // 2

kernel_name = "hybrid_gqa_fnet_moe_dit_prefix"


def rms_norm(x, g):
    x32 = x.astype(jnp.float32)
    y = x32 * lax.rsqrt(jnp.mean(x32 * x32, axis=-1, keepdims=True) + EPS)
    return (y * g.astype(jnp.float32)).astype(x.dtype)


def modulate(h, shift, scale):
    return h * (1 + scale) + shift


def rope_tables(n_tokens):
    n_rows = n_tokens // GRID_W
    rows = jnp.repeat(jnp.arange(n_rows), GRID_W).astype(jnp.float32)
    cols = jnp.tile(jnp.arange(GRID_W), n_rows).astype(jnp.float32)
    inv_freq = ROPE_THETA ** (-jnp.arange(0, ROT_PER_AXIS, 2, dtype=jnp.float32) / ROT_PER_AXIS)
    ang_r = (rows[:, None] * inv_freq)[:, None, :]
    ang_c = (cols[:, None] * inv_freq)[:, None, :]
    return (jnp.cos(ang_r), jnp.sin(ang_r), jnp.cos(ang_c), jnp.sin(ang_c))


def rotate(x, cos, sin):
    x1, x2 = jnp.split(x, 2, axis=-1)
    cos = cos.astype(x.dtype)
    sin = sin.astype(x.dtype)
    return jnp.concatenate([x1 * cos - x2 * sin, x2 * cos + x1 * sin], axis=-1)


def rope2d(x, tables):
    cos_r, sin_r, cos_c, sin_c = tables
    return jnp.concatenate([rotate(x[..., :ROT_PER_AXIS], cos_r, sin_r),
                            rotate(x[..., ROT_PER_AXIS:], cos_c, sin_c)], axis=-1)


def project(h, w_in, g_q, g_k):
    b, n, _ = h.shape
    p = h @ w_in
    q = p[..., :ATTN_WIDTH].reshape(b, n, N_Q_HEADS, HEAD_DIM)
    k = p[..., ATTN_WIDTH:ATTN_WIDTH + KV_WIDTH].reshape(b, n, N_KV_HEADS, HEAD_DIM)
    v = p[..., ATTN_WIDTH + KV_WIDTH:ATTN_WIDTH + 2 * KV_WIDTH].reshape(b, n, N_KV_HEADS, HEAD_DIM)
    f = p[..., ATTN_WIDTH + 2 * KV_WIDTH:]
    return rms_norm(q, g_q), rms_norm(k, g_k), v, f


def attend(q, k, v):
    b, nq = q.shape[:2]
    qg = q.reshape(b, nq, N_KV_HEADS, KV_REP, HEAD_DIM)
    s = jnp.einsum('bqgrd,bkgd->bgrqk', qg, k).astype(jnp.float32) * (HEAD_DIM ** -0.5)
    p = jax.nn.softmax(s, axis=-1).astype(v.dtype)
    o = jnp.einsum('bgrqk,bkgd->bqgrd', p, v)
    return o.reshape(b, nq, ATTN_WIDTH)


def block_attention(q, k, v):
    b, n = q.shape[:2]
    nb = n // Q_BLOCK
    qb = q.reshape(b, nb, Q_BLOCK, N_Q_HEADS, HEAD_DIM).swapaxes(0, 1)
    o = lax.map(lambda qq: attend(qq, k, v), qb)
    return o.swapaxes(0, 1).reshape(b, n, ATTN_WIDTH)


def fourier_mix(f, w_four):
    b, n, _ = f.shape
    fg = f.reshape(b, n, N_FOURIER_GROUPS, FOURIER_GROUP).astype(jnp.float32)
    spec = jnp.fft.fftn(fg, axes=(1, 3), norm='ortho').real.astype(f.dtype)
    y = jnp.einsum('bngc,gcd->bngd', spec, w_four)
    return y.reshape(b, n, FOURIER_WIDTH)


def swiglu(h, w_gate, w_up, w_down):
    return (jax.nn.silu(h @ w_gate) * (h @ w_up)) @ w_down


def moe_swiglu(h, w_router, b_router, w_gate, w_up, w_down):
    t = h.shape[0]
    n_assign = t * TOP_K
    logits = h.astype(jnp.float32) @ w_router.astype(jnp.float32) + b_router.astype(jnp.float32)
    top_val, top_idx = lax.top_k(logits, TOP_K)
    gates = jax.nn.softmax(top_val, axis=-1).reshape(-1)
    expert = top_idx.reshape(-1).astype(jnp.int32)
    token = jnp.repeat(jnp.arange(t, dtype=jnp.int32), TOP_K)
    order = jnp.argsort(expert)
    e_sorted, tok_sorted, gate_sorted = expert[order], token[order], gates[order]
    counts = jnp.bincount(expert, length=N_EXPERTS)
    start = jnp.cumsum(counts) - counts
    padded = ((counts + MOE_BLOCK - 1) // MOE_BLOCK) * MOE_BLOCK
    pend = jnp.cumsum(padded)
    pstart = pend - padded
    dest = pstart[e_sorted] + (jnp.arange(n_assign) - start[e_sorted])
    n_blocks = -(-n_assign // MOE_BLOCK) + N_EXPERTS
    n_rows = n_blocks * MOE_BLOCK
    row_tok = jnp.zeros((n_rows,), jnp.int32).at[dest].set(tok_sorted)
    row_gate = jnp.zeros((n_rows,), jnp.float32).at[dest].set(gate_sorted)
    block_expert = jnp.minimum(
        jnp.searchsorted(pend, jnp.arange(n_blocks) * MOE_BLOCK, side='right'), N_EXPERTS - 1)
    xs = h[row_tok].reshape(n_blocks, MOE_BLOCK, h.shape[-1])

    def expert_block(args):
        xb, e = args
        return swiglu(xb, w_gate[e], w_up[e], w_down[e])

    y = lax.map(expert_block, (xs, block_expert)).reshape(n_rows, h.shape[-1])
    y = y * row_gate[:, None].astype(y.dtype)
    return jax.ops.segment_sum(y, row_tok, num_segments=t)


def setup_inputs(seed: int = 0) -> dict:
    key = jax.random.key(seed)
    ks = jax.random.split(key, 21)
    nrm = lambda k, shape, s: jax.random.normal(k, shape, jnp.float32) * s
    d = D_MODEL
    return {
        "x": nrm(ks[0], (BATCH, SEQ, d), 1.0),
        "c": nrm(ks[1], (BATCH, d), 1.0),
        "ctx": nrm(ks[2], (BATCH, CTX_LEN, d), 1.0),
        "c_ctx": nrm(ks[3], (d,), 1.0),
        "w_mod": nrm(ks[4], (DEPTH, d, N_MOD * d), 0.5 * d ** -0.5),
        "b_mod": nrm(ks[5], (DEPTH, N_MOD * d), 0.02),
        "g_mix": 1.0 + nrm(ks[6], (DEPTH, d), 0.02),
        "g_ffn": 1.0 + nrm(ks[7], (DEPTH, d), 0.02),
        "g_q": 1.0 + nrm(ks[8], (DEPTH, HEAD_DIM), 0.02),
        "g_k": 1.0 + nrm(ks[9], (DEPTH, HEAD_DIM), 0.02),
        "w_in": nrm(ks[10], (DEPTH, d, IN_WIDTH), d ** -0.5),
        "w_four": nrm(ks[11], (DEPTH, N_FOURIER_GROUPS, FOURIER_GROUP, FOURIER_GROUP), FOURIER_GROUP ** -0.5),
        "w_o": nrm(ks[12], (DEPTH, MIX_WIDTH, d), MIX_WIDTH ** -0.5),
        "w_gate_dense": nrm(ks[13], (N_DENSE_LAYERS, d, D_FF_DENSE), d ** -0.5),
        "w_up_dense": nrm(ks[14], (N_DENSE_LAYERS, d, D_FF_DENSE), d ** -0.5),
        "w_down_dense": nrm(ks[15], (N_DENSE_LAYERS, D_FF_DENSE, d), D_FF_DENSE ** -0.5),
        "w_router": nrm(ks[16], (N_MOE_LAYERS, d, N_EXPERTS), d ** -0.5),
        "b_router": nrm(ks[17], (N_MOE_LAYERS, N_EXPERTS), 0.01),
        "w_gate_moe": nrm(ks[18], (N_MOE_LAYERS, N_EXPERTS, d, D_FF_EXPERT), d ** -0.5),
        "w_up_moe": nrm(ks[19], (N_MOE_LAYERS, N_EXPERTS, d, D_FF_EXPERT), d ** -0.5),
        "w_down_moe": nrm(ks[20], (N_MOE_LAYERS, N_EXPERTS, D_FF_EXPERT, d), D_FF_EXPERT ** -0.5),
    }


def reference(x, c, ctx, c_ctx, w_mod, b_mod, g_mix, g_ffn, g_q, g_k, w_in, w_four, w_o,
              w_gate_dense, w_up_dense, w_down_dense, w_router, b_router,
              w_gate_moe, w_up_moe, w_down_moe):
    b, s, d = x.shape
    n_ctx = ctx.shape[1]
    tables = rope_tables(s)
    silu_c = jax.nn.silu(c)
    silu_cc = jax.nn.silu(c_ctx)
    xl, xc = x, ctx
    for l in range(DEPTH):
        last = l == DEPTH - 1
        mod_l = (silu_c @ w_mod[l] + b_mod[l]).reshape(b, N_MOD, 1, d)
        mod_c = (silu_cc @ w_mod[l] + b_mod[l]).reshape(N_MOD, 1, 1, d)
        sh_a, sc_a, ga_a, sh_f, sc_f, ga_f = [mod_l[:, i] for i in range(N_MOD)]
        csh_a, csc_a, cga_a, csh_f, csc_f, cga_f = [mod_c[i] for i in range(N_MOD)]

        hl = modulate(rms_norm(xl, g_mix[l]), sh_a, sc_a)
        hc = modulate(rms_norm(xc, g_mix[l]), csh_a, csc_a)
        ql, kl, vl, fl = project(hl, w_in[l], g_q[l], g_k[l])
        qc, kc, vc, fc = project(hc, w_in[l], g_q[l], g_k[l])
        ql = rope2d(ql, tables)
        kl = rope2d(kl, tables)
        k_all = jnp.concatenate([kc, kl], axis=1)
        v_all = jnp.concatenate([vc, vl], axis=1)
        attn_l = block_attention(ql, k_all, v_all)
        mix_l = jnp.concatenate([attn_l, fourier_mix(fl, w_four[l])], axis=-1) @ w_o[l]
        xl = xl + ga_a * mix_l
        if not last:
            attn_c = attend(qc, kc, vc)
            mix_c = jnp.concatenate([attn_c, fourier_mix(fc, w_four[l])], axis=-1) @ w_o[l]
            xc = xc + cga_a * mix_c

        hl = modulate(rms_norm(xl, g_ffn[l]), sh_f, sc_f).reshape(b * s, d)
        if last:
            tokens = hl
        else:
            hc = modulate(rms_norm(xc, g_ffn[l]), csh_f, csc_f).reshape(b * n_ctx, d)
            tokens = jnp.concatenate([hl, hc], axis=0)
        if l % 2 == 0:
            i = l // 2
            y = swiglu(tokens, w_gate_dense[i], w_up_dense[i], w_down_dense[i])
        else:
            i = l // 2
            y = moe_swiglu(tokens, w_router[i], b_router[i], w_gate_moe[i], w_up_moe[i], w_down_moe[i])
        xl = xl + ga_f * y[:b * s].reshape(b, s, d)
        if not last:
            xc = xc + cga_f * y[b * s:].reshape(b, n_ctx, d)
    return xl
```

```python
import math
from contextlib import ExitStack

import numpy as np
import ml_dtypes
import concourse.bass as bass
import concourse.mybir as mybir
from concourse.bass_utils import run_bass_kernel_spmd

F32 = mybir.dt.float32
BF16 = mybir.dt.bfloat16
AF = mybir.ActivationFunctionType
ALU = mybir.AluOpType
AX = mybir.AxisListType

COMPUTE = ("pe", "act", "dve", "pool")

D = 1024
SEQ = 2048
NCTX = 256
T = SEQ + NCTX
DEPTH = 4
FF_DENSE = 2816
FF_MOE = 3584
NEXP = 8
EPS = 1e-6
TOKCH = [(0, 512), (512, 512), (1024, 512), (1536, 512), (2048, 256)]


class Op:
    __slots__ = ("eng", "fn", "reads", "writes", "is_dma", "deps", "signal",
                 "ticket", "dsem", "dval", "waits", "idx")

    def __init__(self, eng, fn, reads, writes, is_dma):
        self.eng = eng
        self.fn = fn
        self.reads = reads
        self.writes = writes
        self.is_dma = is_dma
        self.deps = set()
        self.signal = False
        self.ticket = None
        self.dsem = None
        self.dval = None
        self.waits = []


class Prog:
    def __init__(self, n_dma_sems=None):
        self.ops = []
        self.last_writer = {}
        self.readers = {}
        self.n_dma_sems = n_dma_sems or {"sp": 24, "pool": 16, "act": 4}

    def _add(self, eng, fn, reads, writes, is_dma):
        op = Op(eng, fn, tuple(reads), tuple(writes), is_dma)
        op.idx = len(self.ops)
        for r in op.reads:
            w = self.last_writer.get(r)
            if w is not None:
                op.deps.add(w)
            self.readers.setdefault(r, []).append(op)
        for r in op.writes:
            w = self.last_writer.get(r)
            if w is not None:
                op.deps.add(w)
            for rd in self.readers.get(r, ()):
                if rd is not op:
                    op.deps.add(rd)
            self.last_writer[r] = op
            self.readers[r] = []
        self.ops.append(op)
        return op

    def op(self, eng, fn, reads=(), writes=()):
        return self._add(eng, fn, reads, writes, False)

    def dma(self, queue, fn, reads=(), writes=()):
        return self._add(queue, fn, reads, writes, True)

    def emit(self, nc, stack):
        engs = {"pe": nc.tensor, "act": nc.scalar, "dve": nc.vector,
                "pool": nc.gpsimd, "sp": nc.sync}
        esem = {e: stack.enter_context(nc.semaphore("s_" + e)) for e in COMPUTE + ("sp",)}
        dsems = {q: [stack.enter_context(nc.semaphore("d_%s%d" % (q, i)))
                     for i in range(n)] for q, n in self.n_dma_sems.items()}
        for op in self.ops:
            best = {}
            keep = set()
            for d in op.deps:
                if d.is_dma:
                    keep.add(d)
                    continue
                if d.eng == "pe" and op.eng == "pe" and not op.is_dma:
                    continue
                b = best.get(d.eng)
                if b is None or b.idx < d.idx:
                    best[d.eng] = d
            keep.update(best.values())
            op.deps = keep
            for d in op.deps:
                d.signal = True
        cnt = {e: 0 for e in esem}
        dcur = {q: 0 for q in dsems}
        dtot = {}
        known = {e: {} for e in engs}
        per_eng = {e: [] for e in engs}
        for op in self.ops:
            waits = {}
            if op.is_dma:
                pool = dsems[op.eng]
                s = pool[dcur[op.eng] % len(pool)]
                dcur[op.eng] += 1
                prev = dtot.get(id(s), 0)
                if prev:
                    waits[id(s)] = (s, prev)
                op.dsem = s
                op.dval = prev + 16
                dtot[id(s)] = op.dval
            elif op.signal:
                cnt[op.eng] += 1
                op.ticket = cnt[op.eng]
            for d in op.deps:
                if d.is_dma:
                    s, v = d.dsem, d.dval
                else:
                    s, v = esem[d.eng], d.ticket
                cur = waits.get(id(s))
                if cur is None or cur[1] < v:
                    waits[id(s)] = (s, v)
            kn = known[op.eng]
            for k, (s, v) in waits.items():
                if kn.get(k, 0) >= v:
                    continue
                kn[k] = v
                op.waits.append((s, v))
            per_eng[op.eng].append(op)
        self.stats = {e: len(v) for e, v in per_eng.items()}
        self.stats["signals"] = dict(cnt)

        def run(eng_name):
            def body(e):
                for op in per_eng[eng_name]:
                    for s, v in op.waits:
                        e.wait_ge(s, v)
                    inst = op.fn(e)
                    if op.is_dma:
                        inst.then_inc(op.dsem, 16)
                    elif op.signal:
                        inst.then_inc(esem[eng_name], 1)
            return body

        with nc.Block() as block:
            block.tensor(run("pe"))
            block.scalar(run("act"))
            block.vector(run("dve"))
            block.gpsimd(run("pool"))
            block.sync(run("sp"))


_CONST_CACHE = {}


def _consts():
    if _CONST_CACHE:
        return _CONST_CACHE
    bf = ml_dtypes.bfloat16
    n = np.arange(SEQ, dtype=np.float64)
    ang = 2.0 * np.pi * ((n[:, None] * n[None, :]) % SEQ) / SEQ
    sc = 1.0 / math.sqrt(SEQ * 128.0)
    C = (np.cos(ang) * sc).astype(np.float32)
    S_ = (np.sin(ang) * sc).astype(np.float32)

    def lay(M):
        return np.ascontiguousarray(M.reshape(16, 128, 8, 256).transpose(2, 1, 0, 3)).astype(bf)

    _CONST_CACHE["dftC"] = lay(C).reshape(8, 128, 4096)
    _CONST_CACHE["dftS"] = lay(S_).reshape(8, 128, 4096)
    m = np.arange(NCTX, dtype=np.float64)
    a2 = 2.0 * np.pi * ((m[:, None] * m[None, :]) % NCTX) / NCTX
    sc2 = 1.0 / math.sqrt(NCTX * 128.0)
    c256 = (np.cos(a2) * sc2).reshape(2, 128, 256).transpose(1, 0, 2)
    s256 = (np.sin(a2) * sc2).reshape(2, 128, 256).transpose(1, 0, 2)
    k = np.arange(128, dtype=np.float64)
    a3 = 2.0 * np.pi * ((k[:, None] * k[None, :]) % 128) / 128.0
    c128 = np.cos(a3)
    s128n = -np.sin(a3)
    inv_freq = 10000.0 ** (-np.arange(0, 32, 2, dtype=np.float64) / 32.0)
    t = np.arange(SEQ)
    rows = (t // 64).astype(np.float64)
    cols = (t % 64).astype(np.float64)
    cos = np.zeros((128, SEQ))
    sin = np.zeros((128, SEQ))
    rmat = np.zeros((128, 128))
    for p in range(128):
        d = p % 64
        pos = rows if d < 32 else cols
        dd = d % 32
        fi = dd % 16
        a = pos * inv_freq[fi]
        cos[p] = np.cos(a)
        sin[p] = np.sin(a) * (-1.0 if dd < 16 else 1.0)
        partner = p + 16 if dd < 16 else p - 16
        rmat[partner, p] = 1.0
    blockones = np.zeros((128, 128))
    blockones[:64, :64] = 1.0
    blockones[64:, 64:] = 1.0
    rope = np.stack([cos, sin], axis=1).reshape(128, 4096)
    small = np.concatenate([
        c256.reshape(128, 512), s256.reshape(128, 512), c128, s128n, rmat, blockones,
        np.ones((128, 128)),
    ], axis=1)
    _CONST_CACHE["rope"] = rope.astype(bf)
    _CONST_CACHE["cbf"] = np.ascontiguousarray(small).astype(bf)
    _CONST_CACHE["ident"] = np.eye(128, dtype=np.float32)
    return _CONST_CACHE


def build(layers, final):
    nc = bass.Bass("TRN2", target_bir_lowering=False)

    def din(name, shape, dt=F32):
        return nc.dram_tensor(name, shape, dt, kind="ExternalInput").ap()

    xT_d = din("xT", [D, T])
    cT_d = din("cT", [128, 8, 2])
    bmod_d = din("bmodT", [128, DEPTH, 48])
    gmix_d = din("gmixT", [128, DEPTH, 8])
    gffn_d = din("gffnT", [128, DEPTH, 8])
    gqk_d = din("gqkT", [128, DEPTH, 2])
    wmod_d = din("w_mod", [DEPTH, D, 6 * D])
    win_d = din("w_in_p", [DEPTH, D, 1280])
    wfour_d = din("w_four", [DEPTH, 4, 128, 128])
    wo_d = din("w_o", [DEPTH, D, D])
    wgd_d = din("w_gate_dense", [2, D, FF_DENSE])
    wud_d = din("w_up_dense", [2, D, FF_DENSE])
    wdd_d = din("w_down_dense", [2, FF_DENSE, D])
    wr_d = din("w_router", [2, D, NEXP])
    br_d = din("b_router", [2, NEXP])
    wgm_d = din("w_gate_moe", [2, NEXP, D, FF_MOE])
    wum_d = din("w_up_moe", [2, NEXP, D, FF_MOE])
    wdm_d = din("w_down_moe", [2, NEXP, FF_MOE, D])
    dftC_d = din("dftC", [8, 128, 4096], BF16)
    dftS_d = din("dftS", [8, 128, 4096], BF16)
    rope_d = din("rope", [128, 4096], BF16)
    cbf_d = din("cbf", [128, 1664], BF16)
    ident_d = din("ident", [128, 128])
    out_d = nc.dram_tensor("out", [D, T], F32, kind="ExternalOutput").ap()

    P = Prog()
    with ExitStack() as st:
        def sb(name, shape, dt=F32):
            return st.enter_context(nc.sbuf_tensor(name, shape, dt))

        x = sb("x", [128, 8, T])
        BIG = sb("BIG", [128, 18432], BF16)
        ring = [sb("ring%d" % i, [128, 4096], BF16) for i in range(6)]
        HC = [sb("HC%d" % i, [128, 8, 512], BF16) for i in range(2)]
        kT = sb("kT", [128, T], BF16)
        V = sb("V", [128, 18, 128], BF16)
        PT = [sb("PT%d" % i, [128, 512], BF16) for i in range(4)]
        TF = [sb("TF%d" % i, [128, 512], F32) for i in range(3)]
        TB = [sb("TB%d" % i, [128, 512], BF16) for i in range(3)]
        cbf = sb("cbf_s", [128, 1664], BF16)
        ident = sb("ident_s", [128, 128], F32)
        AB = sb("AB", [128, 4, 256], BF16)
        WF = sb("WF", [128, 4, 128], BF16)
        cT = sb("cT_s", [128, 8, 2], F32)
        siluc = sb("siluc", [128, 8, 2], BF16)
        bmod = sb("bmod", [128, DEPTH, 48], F32)
        gmix = sb("gmix", [128, DEPTH, 8], F32)
        gffn = sb("gffn", [128, DEPTH, 8], F32)
        gqk = sb("gqk", [128, DEPTH, 2], F32)
        modT = sb("modT", [128, 48, 2], F32)
        G1 = sb("G1", [128, 8, 2], F32)
        G2 = sb("G2", [128, 8, 2], F32)
        wr32 = sb("wr32", [128, 8, 8], F32)
        wrd = sb("wrd", [128, 8, 8], F32)
        wrhi = sb("wrhi", [128, 8, 8], BF16)
        wrlo = sb("wrlo", [128, 8, 8], BF16)
        brt = sb("brt", [128, 8], F32)
        Lg = sb("Lg", [128, 18, 8], F32)
        Lq = sb("Lq", [128, 18, 8], F32)
        Gt = sb("Gt", [128, 18, 8], F32)
        m1 = sb("m1", [128, 18], F32)
        m2 = sb("m2", [128, 18], F32)
        ps = [st.enter_context(nc.psum_tensor("ps%d" % i, [128, 512], F32)) for i in range(8)]

        c256 = cbf[:, 0:512].rearrange("p (i n) -> p i n", i=2)
        s256 = cbf[:, 512:1024].rearrange("p (i n) -> p i n", i=2)
        c128 = cbf[:, 1024:1152]
        s128n = cbf[:, 1152:1280]
        rmat = cbf[:, 1280:1408]
        blockones = cbf[:, 1408:1536]
        ones128 = cbf[:, 1536:1664]
        ones64 = cbf[:, 1536:1600]

        qT = BIG[:, 0:9216].rearrange("p (c t) -> p c t", c=4)
        fTM = BIG[:, 9216:18432].rearrange("p (i n) -> p i n", n=512)
        h2 = BIG[:, :].rearrange("p (c t) -> p c t", c=8)
        Gb = kT

        def bk(col0, ncols):
            return [("B", b) for b in range(col0 // 256, (col0 + ncols + 255) // 256)]

        def qkeys(c, t0, tw):
            return bk(c * T + t0, tw)

        def fkeys(i):
            return bk(9216 + i * 512, 512)

        def hkeys(c, t0, tw):
            return bk(c * T + t0, tw)

        bank_roles = {}
        bank_cnt = {}

        def set_roles(**roles):
            bank_roles.clear()
            bank_roles.update(roles)
            bank_cnt.clear()

        def bank(role):
            lst = bank_roles[role]
            n = bank_cnt.get(role, 0)
            bank_cnt[role] = n + 1
            b = lst[n % len(lst)]
            return ps[b], ("ps", b)

        def mm(out, lhsT, rhs, start, stop, reads, writes, **kw):
            P.op("pe", lambda e: e.matmul(out, lhsT=lhsT, rhs=rhs, start=start, stop=stop, **kw),
                 reads, writes)

        def act(out, in_, func, reads, writes, **kw):
            P.op("act", lambda e: e.activation(out=out, in_=in_, func=func, **kw), reads, writes)

        def tt(eng, out, in0, in1, op, reads, writes):
            P.op(eng, lambda e: e.tensor_tensor(out=out, in0=in0, in1=in1, op=op), reads, writes)

        def stt(out, in0, scalar, in1, op0, op1, reads, writes):
            P.op("dve", lambda e: e.scalar_tensor_tensor(out=out, in0=in0, scalar=scalar, in1=in1,
                                                          op0=op0, op1=op1), reads, writes)

        def tsc(eng, out, in0, s1, s2, op0, op1, reads, writes):
            P.op(eng, lambda e: e.tensor_scalar(out=out, in0=in0, scalar1=s1, scalar2=s2, op0=op0, op1=op1),
                 reads, writes)

        def recip(out, in_, reads, writes):
            P.op("dve", lambda e: e.reciprocal(out=out, in_=in_), reads, writes)

        def cpy(eng, out, in_, reads, writes):
            if eng == "act":
                act(out, in_, AF.Copy, reads, writes)
            else:
                P.op(eng, lambda e: e.tensor_copy(out=out, in_=in_), reads, writes)

        def red(out, in_, op, reads, writes):
            P.op("dve", lambda e: e.tensor_reduce(out=out, in_=in_, op=op, axis=AX.X), reads, writes)

        def dma(q, out, in_, reads, writes):
            P.dma(q, lambda e: e.dma_start(out=out, in_=in_), reads, writes)

        xk = lambda o, j: ("x", o, j)
        allx = [xk(o, j) for o in range(8) for j in range(5)]

        dma("sp", cbf[:], cbf_d, [], ["cbf"])
        dma("sp", ident[:], ident_d, [], ["ident"])
        dma("sp", cT[:], cT_d, [], ["cT"])
        dma("sp", bmod[:], bmod_d, [], ["bmod"])
        dma("sp", gmix[:], gmix_d, [], ["gmix"])
        dma("sp", gffn[:], gffn_d, [], ["gffn"])
        dma("sp", gqk[:], gqk_d, [], ["gqk"])
        xv = xT_d.rearrange("(c p) t -> p c t", p=128)
        for c in range(8):
            dma("sp", x[:, c, :], xv[:, c, :], [], [xk(c, j) for j in range(5)])
        act(siluc[:], cT[:], AF.Silu, ["cT"], ["siluc"])

        for l in layers:
            last = final and (l == DEPTH - 1)
            moe = (l % 2 == 1)
            li = l // 2
            chunks = [(j, t0, tw) for j, (t0, tw) in enumerate(TOKCH)]
            upd_chunks = [c_ for c_ in chunks if not (last and c_[0] == 4)]

            set_roles(mod=[0])
            wmv = wmod_d[l].rearrange("(k p) n -> p k n", p=128)
            mb, mbk = bank("mod")
            for s in range(12):
                slot = ring[4 + s % 2]
                sk = ("R", 4 + s % 2)
                sv = slot[:, :].rearrange("p (k n) -> p k n", k=8)
                dma("pool", sv, wmv[:, :, s * 512:(s + 1) * 512], [], [sk])
                for jj in range(4):
                    j = s * 4 + jj
                    for k in range(8):
                        mm(mb[:, 2 * j:2 * j + 2], sv[:, k, jj * 128:(jj + 1) * 128], siluc[:, k, :],
                           k == 0, k == 7, [sk, "siluc"], [mbk])
            tt("dve", modT[:], mb[:, 0:96].rearrange("p (j s) -> p j s", s=2),
               bmod[:, l, :].unsqueeze(2).to_broadcast([128, 48, 2]), ALU.add, [mbk, "bmod"], ["modT"])
            tsc("dve", G1[:], modT[:, 8:16, :], 1.0, None, ALU.add, ALU.bypass, ["modT"], ["G1"])
            tt("dve", G1[:], G1[:], gmix[:, l, :].unsqueeze(2).to_broadcast([128, 8, 2]), ALU.mult,
               ["G1", "gmix"], ["G1"])
            tsc("dve", G2[:], modT[:, 32:40, :], 1.0, None, ALU.add, ALU.bypass, ["modT"], ["G2"])
            tt("dve", G2[:], G2[:], gffn[:, l, :].unsqueeze(2).to_broadcast([128, 8, 2]), ALU.mult,
               ["G2", "gffn"], ["G2"])

            set_roles(ss=[0, 1], qk=[2, 3], qss=[4], rot=[5], vf=[6, 7])
            Wq = ring[0][:, :].rearrange("p (k n) -> p k n", k=8)
            Wkv = ring[1][:, 0:2048].rearrange("p (k n) -> p k n", k=8)
            Wf = ring[2][:, :].rearrange("p (k n) -> p k n", k=8)
            wiv = win_d[l].rearrange("(k p) n -> p k n", p=128)
            dma("pool", Wq, wiv[:, :, 0:512], [], [("R", 0)])
            dma("pool", Wkv, wiv[:, :, 512:768], [], [("R", 1)])
            dma("pool", Wf, wiv[:, :, 768:1280], [], [("R", 2)])
            dma("sp", ring[3][:, :], rope_d, [], [("R", 3)])
            COS = ring[3][:, 0:2048]
            SIN = ring[3][:, 2048:4096]

            def norm_chunk(j, t0, tw, Gs, gkey, shbase, dest_fn, dest_keys_fn):
                s = 1 if j == 4 else 0
                hp = j % 2
                hc = HC[hp]
                hk = ("HC", hp)
                act(hc[:, :, :tw], x[:, :, t0:t0 + tw], AF.Square, [xk(c, j) for c in range(8)], [hk])
                sbk, sbkk = bank("ss")
                for c in range(8):
                    mm(sbk[:, :tw], ones128, hc[:, c, :tw], c == 0, c == 7, [hk, "cbf"], [sbkk])
                act(TF[0][:, :tw], sbk[:, :tw], AF.Sqrt, [sbkk], ["TF0"], scale=1.0 / D, bias=EPS)
                recip(TF[0][:, :tw], TF[0][:, :tw], ["TF0"], ["TF0"])
                for c in range(8):
                    tmp = TF[1 + c % 2]
                    tk = "TF%d" % (1 + c % 2)
                    stt(tmp[:, :tw], x[:, c, t0:t0 + tw], Gs[:, c, s:s + 1], TF[0][:, :tw], ALU.mult, ALU.mult,
                        [xk(c, j), gkey, "TF0"], [tk])
                    act(dest_fn(c), tmp[:, :tw], AF.Identity, [tk, "modT"], dest_keys_fn(c),
                        bias=modT[:, shbase + c, s:s + 1], scale=1.0)

            def qk_post(pb, pbk, tw, gcol, dest, destkeys, rope, t0):
                act(TB[0][:, :tw], pb[:, :tw], AF.Square, [pbk], ["TB0"])
                sbk, sbkk = bank("qss")
                mm(sbk[:, :tw], blockones, TB[0][:, :tw], True, True, ["TB0", "cbf"], [sbkk])
                act(TF[0][:, :tw], sbk[:, :tw], AF.Sqrt, [sbkk], ["TF0"], scale=1.0 / 64.0, bias=EPS)
                recip(TF[0][:, :tw], TF[0][:, :tw], ["TF0"], ["TF0"])
                g = gqk[:, l, gcol:gcol + 1]
                if rope:
                    stt(TB[1][:, :tw], pb[:, :tw], g, TF[0][:, :tw], ALU.mult, ALU.mult,
                        [pbk, "TF0", "gqk"], ["TB1"])
                    rb, rbk = bank("rot")
                    mm(rb[:, :tw], rmat, TB[1][:, :tw], True, True, ["TB1", "cbf"], [rbk])
                    tt("pool", TF[1][:, :tw], TB[1][:, :tw], COS[:, t0:t0 + tw], ALU.mult,
                       ["TB1", ("R", 3)], ["TF1"])
                    tt("dve", TF[2][:, :tw], rb[:, :tw], SIN[:, t0:t0 + tw], ALU.mult,
                       [rbk, ("R", 3)], ["TF2"])
                    tt("pool", dest, TF[1][:, :tw], TF[2][:, :tw], ALU.add, ["TF1", "TF2"], destkeys)
                else:
                    stt(dest, pb[:, :tw], g, TF[0][:, :tw], ALU.mult, ALU.mult,
                        [pbk, "TF0", "gqk"], destkeys)

            for (j, t0, tw) in chunks:
                hp = j % 2
                hc = HC[hp]
                hk = ("HC", hp)
                norm_chunk(j, t0, tw, G1, "G1", 0, lambda c: hc[:, c, :tw], lambda c: [hk])
                need_qf = not (last and j == 4)
                if need_qf:
                    for c in range(4):
                        pb, pbk = bank("qk")
                        for k in range(8):
                            mm(pb[:, :tw], Wq[:, k, c * 128:(c + 1) * 128], hc[:, k, :tw], k == 0, k == 7,
                               [("R", 0), hk], [pbk])
                        qk_post(pb, pbk, tw, 0, qT[:, c, t0:t0 + tw], qkeys(c, t0, tw), j < 4, t0)
                pb, pbk = bank("qk")
                for k in range(8):
                    mm(pb[:, :tw], Wkv[:, k, 0:128], hc[:, k, :tw], k == 0, k == 7, [("R", 1), hk], [pbk])
                qk_post(pb, pbk, tw, 1, kT[:, t0:t0 + tw], [("kT", j)], j < 4, t0)
                nt = tw // 128
                i0 = t0 // 128
                pb, pbk = bank("vf")
                for it in range(nt):
                    for k in range(8):
                        mm(pb[:, it * 128:(it + 1) * 128], hc[:, k, it * 128:(it + 1) * 128], Wkv[:, k, 128:256],
                           k == 0, k == 7, [("R", 1), hk], [pbk])
                cpy("act", V[:, i0:i0 + nt, :], pb[:, 0:nt * 128].rearrange("p (i n) -> p i n", n=128),
                    [pbk], [("V", i0 + it) for it in range(nt)])
                if need_qf:
                    for it in range(nt):
                        pb, pbk = bank("vf")
                        for k in range(8):
                            mm(pb[:, :], hc[:, k, it * 128:(it + 1) * 128], Wf[:, k, :], k == 0, k == 7,
                               [("R", 2), hk], [pbk])
                        cpy("dve" if it % 2 else "act", fTM[:, i0 + it, :], pb[:, :], [pbk], fkeys(i0 + it))

            set_roles(cf=[0, 1], sf=[2, 3], y=[4, 5], op=[6, 7])
            dma("pool", WF[:], wfour_d[l].rearrange("g c d -> c g d"), [], ["WF"])
            woF = ring[5][:, :].rearrange("p (g n) -> p g n", g=4)
            dma("pool", woF, wo_d[l, 512:1024, :].rearrange("(g p) n -> p g n", p=128), [], [("R", 5)])
            woA = ring[4][:, :].rearrange("p (c n) -> p c n", c=4)
            for m_ in range(2):
                dma("pool", ring[4][64 * m_:64 * m_ + 64, :].rearrange("p (c n) -> p c n", c=4),
                    wo_d[l, 256 * m_:256 * m_ + 256, :].rearrange("(c d) n -> d c n", d=64), [], [("R", 4)])
            yb, ybk = bank("y")
            for g in range(4):
                mm(yb[:, g * 128:(g + 1) * 128], c128, WF[:, g, :], True, True, ["cbf", "WF"], [ybk])
            cpy("act", AB[:, :, 0:128], yb[:, :].rearrange("p (g n) -> p g n", g=4), [ybk], ["AB"])
            yb, ybk = bank("y")
            for g in range(4):
                mm(yb[:, g * 128:(g + 1) * 128], s128n, WF[:, g, :], True, True, ["cbf", "WF"], [ybk])
            cpy("act", AB[:, :, 128:256], yb[:, :].rearrange("p (g n) -> p g n", g=4), [ybk], ["AB"])

            def fourier_chunk(n0, nw, jx, s, tiles, Ct, St, tabkeys, stg, stgk):
                for g in range(4):
                    cb, cbk = bank("cf")
                    sbk, sbkk = bank("sf")
                    nt_ = len(tiles)
                    for n_, i in enumerate(tiles):
                        mm(cb[:, :nw], fTM[:, i, g * 128:(g + 1) * 128], Ct[:, n_, :], n_ == 0, n_ == nt_ - 1,
                           fkeys(i) + tabkeys, [cbk])
                    for n_, i in enumerate(tiles):
                        mm(sbk[:, :nw], fTM[:, i, g * 128:(g + 1) * 128], St[:, n_, :], n_ == 0, n_ == nt_ - 1,
                           fkeys(i) + tabkeys, [sbkk])
                    cpy("act", TB[0][:, :nw], cb[:, :nw], [cbk], ["TB0"])
                    cpy("dve", TB[1][:, :nw], sbk[:, :nw], [sbkk], ["TB1"])
                    yb, ybk = bank("y")
                    mm(yb[:, :nw], AB[:, g, 0:128], TB[0][:, :nw], True, False, ["AB", "TB0"], [ybk])
                    mm(yb[:, :nw], AB[:, g, 128:256], TB[1][:, :nw], False, True, ["AB", "TB1"], [ybk])
                    cpy("act", stg[:, g, :nw], yb[:, :nw], [ybk], [stgk])
                for o in range(8):
                    ob, obk = bank("op")
                    for g in range(4):
                        mm(ob[:, :nw], woF[:, g, o * 128:(o + 1) * 128], stg[:, g, :nw], g == 0, g == 3,
                           [("R", 5), stgk], [obk])
                    stt(x[:, o, n0:n0 + nw], ob[:, :nw], modT[:, 16 + o, s:s + 1], x[:, o, n0:n0 + nw],
                        ALU.mult, ALU.add, [obk, "modT", xk(o, jx)], [xk(o, jx)])

            for jj in range(8):
                a_ = (jj % 2) * 2
                Ct = ring[a_][:, :].rearrange("p (i n) -> p i n", i=16)
                St = ring[a_ + 1][:, :].rearrange("p (i n) -> p i n", i=16)
                dma("sp", ring[a_][:, :], dftC_d[jj], [], [("R", a_)])
                dma("sp", ring[a_ + 1][:, :], dftS_d[jj], [], [("R", a_ + 1)])
                stg = HC[jj % 2][:, 0:2, :].rearrange("p a (b n) -> p (a b) n", b=2)
                fourier_chunk(jj * 256, 256, jj // 2, 0, list(range(16)), Ct, St,
                              [("R", a_), ("R", a_ + 1)], stg, ("HC", jj % 2))
            if not last:
                stg = HC[0][:, 0:2, :].rearrange("p a (b n) -> p (a b) n", b=2)
                fourier_chunk(SEQ, 256, 4, 1, [16, 17], c256, s256, ["cbf"], stg, ("HC", 0))

            set_roles(sc=[0, 1, 2, 3], op=[6, 7])
            Ob, Obk = ps[4], ("ps", 4)
            Db, Dbk = ps[5], ("ps", 5)
            ptc = [0]

            def nextpt():
                i = ptc[0] % 4
                ptc[0] += 1
                return PT[i], "PT%d" % i

            for (j, t0, tw) in upd_chunks:
                s = 1 if j == 4 else 0
                ktiles = list(range(18)) if j < 4 else [16, 17]
                stg = HC[j % 2][:, 0:4, :]
                stgk = ("HC", j % 2)
                for c in range(4):
                    qk_ = qkeys(c, t0, tw)

                    def scores(i):
                        kk = ("kT", 4 if i >= 16 else i // 4)
                        s1, s1k = bank("sc")
                        s2, s2k = bank("sc")
                        mm(s1[:, :tw], kT[0:64, i * 128:(i + 1) * 128], qT[0:64, c, t0:t0 + tw], True, True,
                           [kk] + qk_, [s1k])
                        mm(s2[:, :tw], kT[64:128, i * 128:(i + 1) * 128], qT[64:128, c, t0:t0 + tw], True, True,
                           [kk] + qk_, [s2k])
                        return s1, s1k, s2, s2k

                    nxt = scores(ktiles[0])
                    for n_, i in enumerate(ktiles):
                        s1, s1k, s2, s2k = nxt
                        if n_ + 1 < len(ktiles):
                            nxt = scores(ktiles[n_ + 1])
                        p1, p1k = nextpt()
                        p2, p2k = nextpt()
                        act(p1[:, :tw], s1[:, :tw], AF.Exp, [s1k], [p1k], scale=0.125)
                        act(p2[:, :tw], s2[:, :tw], AF.Exp, [s2k], [p2k], scale=0.125)
                        st_, sp_ = (n_ == 0), (n_ == len(ktiles) - 1)
                        mm(Ob[0:64, :tw], V[:, i, 0:64], p1[:, :tw], st_, sp_, [("V", i), p1k], [Obk],
                           tile_position=(0, 0))
                        mm(Ob[64:128, :tw], V[:, i, 64:128], p2[:, :tw], st_, sp_, [("V", i), p2k], [Obk],
                           tile_position=(0, 64))
                        mm(Db[0:64, :tw], ones64, p1[:, :tw], st_, sp_, ["cbf", p1k], [Dbk],
                           tile_position=(0, 0))
                        mm(Db[64:128, :tw], ones64, p2[:, :tw], st_, sp_, ["cbf", p2k], [Dbk],
                           tile_position=(0, 64))
                    recip(TF[0][:, :tw], Db[:, :tw], [Dbk], ["TF0"])
                    tt("dve", stg[:, c, :tw], Ob[:, :tw], TF[0][:, :tw], ALU.mult, [Obk, "TF0"], [stgk])
                for o in range(8):
                    ob, obk = bank("op")
                    for c in range(4):
                        mm(ob[:, :tw], woA[:, c, o * 128:(o + 1) * 128], stg[:, c, :tw], c == 0, c == 3,
                           [("R", 4), stgk], [obk])
                    stt(x[:, o, t0:t0 + tw], ob[:, :tw], modT[:, 16 + o, s:s + 1], x[:, o, t0:t0 + tw],
                        ALU.mult, ALU.add, [obk, "modT", xk(o, j)], [xk(o, j)])

            set_roles(ss=[0, 1])
            for (j, t0, tw) in upd_chunks:
                norm_chunk(j, t0, tw, G2, "G2", 24, lambda c: h2[:, c, t0:t0 + tw], lambda c: hkeys(c, t0, tw))
            tiles_upd = list(range(16)) if last else list(range(18))

            if moe:
                set_roles(lg=[7])
                dma("sp", wr32[:], wr_d[li].rearrange("(k p) e -> p k e", p=128), [], ["wr32"])
                dma("sp", brt[:], br_d[li].partition_broadcast(128), [], ["brt"])
                cpy("dve", wrhi[:], wr32[:], ["wr32"], ["wrhi"])
                tt("dve", wrd[:], wr32[:], wrhi[:], ALU.subtract, ["wr32", "wrhi"], ["wrd"])
                cpy("dve", wrlo[:], wrd[:], ["wrd"], ["wrlo"])
                lb, lbk = bank("lg")
                for i in tiles_upd:
                    t0i = i * 128
                    jch = 4 if i >= 16 else i // 4
                    rk = [k_ for c in range(8) for k_ in hkeys(c, t0i, 128)]
                    for k in range(8):
                        mm(lb[:, i * 8:(i + 1) * 8], h2[:, k, t0i:t0i + 128], wrhi[:, k, :], k == 0, False,
                           rk + ["wrhi"], [lbk])
                    for k in range(8):
                        mm(lb[:, i * 8:(i + 1) * 8], h2[:, k, t0i:t0i + 128], wrlo[:, k, :], False, k == 7,
                           rk + ["wrlo"], [lbk])
                nt_ = len(tiles_upd)
                Lv = Lg[:, 0:nt_, :]
                Qv = Lq[:, 0:nt_, :]
                Gv = Gt[:, 0:nt_, :]
                bshape = [128, nt_, 8]
                tt("dve", Lv, lb[:, 0:nt_ * 8].rearrange("p (i e) -> p i e", e=8),
                   brt[:].unsqueeze(1).to_broadcast(bshape), ALU.add, [lbk, "brt"], ["Lg"])
                red(m1[:, 0:nt_], Lv, ALU.max, ["Lg"], ["m1"])
                tt("dve", Qv, Lv, m1[:, 0:nt_].unsqueeze(2).to_broadcast(bshape), ALU.is_ge, ["Lg", "m1"], ["Lq"])
                stt(Qv, Qv, -1.0e30, Lv, ALU.mult, ALU.add, ["Lq", "Lg"], ["Lq"])
                red(m2[:, 0:nt_], Qv, ALU.max, ["Lq"], ["m2"])
                tt("dve", Qv, Lv, m2[:, 0:nt_].unsqueeze(2).to_broadcast(bshape), ALU.is_ge, ["Lg", "m2"], ["Lq"])
                tt("dve", Gv, Lv, m1[:, 0:nt_].unsqueeze(2).to_broadcast(bshape), ALU.subtract,
                   ["Lg", "m1"], ["Gt"])
                act(Gv, Gv, AF.Exp, ["Gt"], ["Gt"])
                tt("dve", Gv, Gv, Qv, ALU.mult, ["Gt", "Lq"], ["Gt"])
                red(m2[:, 0:nt_], Gv, ALU.add, ["Gt"], ["m2"])
                recip(m2[:, 0:nt_], m2[:, 0:nt_], ["m2"], ["m2"])
                tt("dve", Gv, Gv, m2[:, 0:nt_].unsqueeze(2).to_broadcast(bshape), ALU.mult, ["Gt", "m2"], ["Gt"])

            set_roles(g=[0, 1], u=[2, 3], dn=[4, 5, 6], gb=[7])
            if moe:
                ffw = FF_MOE
                units = [(e, f0, min(512, ffw - f0)) for e in range(NEXP) for f0 in range(0, ffw, 512)]
            else:
                ffw = FF_DENSE
                units = [(None, f0, min(512, ffw - f0)) for f0 in range(0, ffw, 512)]

            def load_unit(n_):
                e, f0, fw = units[n_]
                A = (n_ % 2) * 3
                if moe:
                    wg, wu, wd = wgm_d[li, e], wum_d[li, e], wdm_d[li, e]
                else:
                    wg, wu, wd = wgd_d[li], wud_d[li], wdd_d[li]
                nfc = fw // 128
                Wg = ring[A][:, 0:8 * fw].rearrange("p (k n) -> p k n", k=8)
                Wu = ring[A + 1][:, 0:8 * fw].rearrange("p (k n) -> p k n", k=8)
                Wd = ring[A + 2][:, 0:nfc * 1024].rearrange("p (c n) -> p c n", c=nfc)
                dma("pool", Wg, wg.rearrange("(k p) n -> p k n", p=128)[:, :, f0:f0 + fw], [], [("R", A)])
                dma("pool", Wu, wu.rearrange("(k p) n -> p k n", p=128)[:, :, f0:f0 + fw], [], [("R", A + 1)])
                dma("pool", Wd, wd[f0:f0 + fw, :].rearrange("(c p) n -> p c n", p=128), [], [("R", A + 2)])
                return Wg, Wu, Wd, A, nfc

            loaded = load_unit(0)
            cnt_ab = 0
            cnt_tb = 0
            for n_, (e, f0, fw) in enumerate(units):
                Wg, Wu, Wd, A, nfc = loaded
                if n_ + 1 < len(units):
                    loaded = load_unit(n_ + 1)
                if moe and f0 == 0:
                    for i4 in range(0, len(tiles_upd), 4):
                        grp = tiles_upd[i4:i4 + 4]
                        gb, gbk = bank("gb")
                        for q_, i in enumerate(grp):
                            mm(gb[:, q_ * 128:(q_ + 1) * 128], Gt[:, i, e:e + 1].to_broadcast([128, 128]), ident[:],
                               True, True, ["Gt", "ident"], [gbk])
                        w_ = len(grp) * 128
                        cpy("act", Gb[:, grp[0] * 128:grp[0] * 128 + w_], gb[:, 0:w_], [gbk],
                            [("kT", 4 if grp[0] >= 16 else grp[0] // 4)])
                for (j, t0, tw) in upd_chunks:
                    s = 1 if j == 4 else 0
                    ab = HC[cnt_ab % 2]
                    abk = ("HC", cnt_ab % 2)
                    cnt_ab += 1
                    hk_all = [k_ for c in range(8) for k_ in hkeys(c, t0, tw)]
                    for fc in range(nfc):
                        gbn, gbnk = bank("g")
                        ubn, ubnk = bank("u")
                        for k in range(8):
                            mm(gbn[:, :tw], Wg[:, k, fc * 128:(fc + 1) * 128], h2[:, k, t0:t0 + tw], k == 0, k == 7,
                               [("R", A)] + hk_all, [gbnk])
                        for k in range(8):
                            mm(ubn[:, :tw], Wu[:, k, fc * 128:(fc + 1) * 128], h2[:, k, t0:t0 + tw], k == 0, k == 7,
                               [("R", A + 1)] + hk_all, [ubnk])
                        sg = TB[cnt_tb % 3]
                        sgk = "TB%d" % (cnt_tb % 3)
                        cnt_tb += 1
                        act(sg[:, :tw], gbn[:, :tw], AF.Silu, [gbnk], [sgk])
                        if moe:
                            tt("pool", sg[:, :tw], sg[:, :tw], Gb[:, t0:t0 + tw], ALU.mult,
                               [sgk, ("kT", j)], [sgk])
                        tt("dve", ab[:, fc, :tw], ubn[:, :tw], sg[:, :tw], ALU.mult, [ubnk, sgk], [abk])
                    for o in range(8):
                        db, dbk = bank("dn")
                        for fc in range(nfc):
                            mm(db[:, :tw], Wd[:, fc, o * 128:(o + 1) * 128], ab[:, fc, :tw], fc == 0, fc == nfc - 1,
                               [("R", A + 2), abk], [dbk])
                        stt(x[:, o, t0:t0 + tw], db[:, :tw], modT[:, 40 + o, s:s + 1], x[:, o, t0:t0 + tw],
                            ALU.mult, ALU.add, [dbk, "modT", xk(o, j)], [xk(o, j)])

        ov = out_d.rearrange("(c p) t -> p c t", p=128)
        for c in range(8):
            dma("sp", ov[:, c, :], x[:, c, :], [xk(c, j) for j in range(5)], [("out", c)])
        P.op("sp", lambda e: e.nop(), [("out", c) for c in range(8)], [])
        P.emit(nc, st)
    build.stats = P.stats
    return nc


LAYER_GROUPS = [[0, 1, 2, 3]]

_QPERM = np.array([(c + 4 * m) * 64 + d for c in range(4) for m in range(2) for d in range(64)])


def _prep_shared(inp):
    f = lambda a: np.ascontiguousarray(np.asarray(a, dtype=np.float32))
    sh = {}
    w_in = np.asarray(inp["w_in"], dtype=np.float32)
    perm = np.concatenate([_QPERM, np.arange(512, 1280)])
    sh["w_in_p"] = np.ascontiguousarray(w_in[:, :, perm])
    for k in ("w_mod", "w_four", "w_o", "w_gate_dense", "w_up_dense", "w_down_dense", "w_router", "b_router",
              "w_gate_moe", "w_up_moe", "w_down_moe"):
        sh[k] = f(inp[k])
    sh["bmodT"] = np.ascontiguousarray(np.asarray(inp["b_mod"], np.float32).reshape(DEPTH, 48, 128).transpose(2, 0, 1))
    sh["gmixT"] = np.ascontiguousarray(np.asarray(inp["g_mix"], np.float32).reshape(DEPTH, 8, 128).transpose(2, 0, 1))
    sh["gffnT"] = np.ascontiguousarray(np.asarray(inp["g_ffn"], np.float32).reshape(DEPTH, 8, 128).transpose(2, 0, 1))
    gq = np.asarray(inp["g_q"], np.float32)
    gk = np.asarray(inp["g_k"], np.float32)
    gqk = np.stack([np.tile(gq, (1, 2)), np.tile(gk, (1, 2))], axis=-1)
    sh["gqkT"] = np.ascontiguousarray(gqk.transpose(1, 0, 2))
    cs = _consts()
    for k in ("dftC", "dftS", "rope", "cbf", "ident"):
        sh[k] = cs[k]
    return sh


def kernel(**inputs):
    x = np.asarray(inputs["x"], np.float32)
    c = np.asarray(inputs["c"], np.float32)
    ctx = np.asarray(inputs["ctx"], np.float32)
    c_ctx = np.asarray(inputs["c_ctx"], np.float32)
    B = x.shape[0]
    shared = _prep_shared(inputs)
    xT = [np.ascontiguousarray(np.concatenate([x[b], ctx[b]], axis=0).T) for b in range(B)]
    cT = []
    for b in range(B):
        cc = np.stack([c[b].reshape(8, 128).T, c_ctx.reshape(8, 128).T], axis=-1)
        cT.append(np.ascontiguousarray(cc))
    for gi, layers in enumerate(LAYER_GROUPS):
        nc = build(layers, final=True)
        in_maps = []
        for b in range(B):
            m = dict(shared)
            m["xT"] = xT[b]
            m["cT"] = cT[b]
            in_maps.append(m)
        res = run_bass_kernel_spmd(nc, in_maps, core_ids=list(range(B)))
        xT = [np.asarray(res.results[b]["out"], np.float32) for b in range(B)]
    out = np.stack([xT[b][:, :SEQ].T for b in range(B)], axis=0)
    return np.ascontiguousarray(out.astype(np.float32))
```
